# Optimizing a Trainium2 kernel written in Bass

```python
import math
import jax, jax.numpy as jnp
from jax import lax
import numpy as np

D_MODEL = 4096
BATCH = 4
SEQ = 4096
DEPTH = 2

MIX_WIDTH = D_MODEL
NORM_EPS = 1e-5
SB_HEAD_DIM = 128
SB_WIDTH = MIX_WIDTH // 4
SB_HEADS = SB_WIDTH // SB_HEAD_DIM
SB_BLOCK = 128
HG_EXPAND = 128
HG_WIDTH = MIX_WIDTH // 4
HG_HEADS = HG_WIDTH // HG_EXPAND
HG_HEAD_V = HG_WIDTH // HG_HEADS
HG_CHUNK = 64
HG_MIN_F = 1e-30
RW_HEAD_DIM = 64
RW_WIDTH = MIX_WIDTH - SB_WIDTH - HG_WIDTH
RW_HEADS = RW_WIDTH // RW_HEAD_DIM
RW_DECAY_RANK = max(32, int(round(math.sqrt(RW_WIDTH) * 1.8 / 32)) * 32)
RW_AAA_RANK = max(32, int(round(math.sqrt(RW_WIDTH) * 1.8 / 32)) * 32)
RW_GATE_RANK = max(32, int(round((RW_WIDTH ** 0.6) * 0.8 / 32)) * 32)
RW_GN_EPS = 64e-5
RW_COLS = 3 * RW_WIDTH + RW_DECAY_RANK + RW_AAA_RANK + RW_GATE_RANK
D_FF = 4 * D_MODEL
IN_COLS = 3 * SB_WIDTH + 4 * HG_WIDTH + RW_COLS
IN_SPLITS = [SB_WIDTH, 2 * SB_WIDTH, 3 * SB_WIDTH,
             3 * SB_WIDTH + HG_WIDTH, 3 * SB_WIDTH + 2 * HG_WIDTH,
             3 * SB_WIDTH + 3 * HG_WIDTH, 3 * SB_WIDTH + 4 * HG_WIDTH]
RW_SPLITS = [RW_WIDTH, 2 * RW_WIDTH, 3 * RW_WIDTH,
             3 * RW_WIDTH + RW_DECAY_RANK, 3 * RW_WIDTH + RW_DECAY_RANK + RW_AAA_RANK]

kernel_name = "hymba_style_sb_hgrn2_rwkv7_hybrid"


def rms_norm(x, g, eps=NORM_EPS):
    xf = x.astype(jnp.float32)
    y = xf * lax.rsqrt(jnp.mean(xf * xf, axis=-1, keepdims=True) + eps)
    return (y * g.astype(jnp.float32)).astype(x.dtype)


def stick_breaking_attention(q, k, v):
    T = q.shape[1]
    qh = jnp.swapaxes(q, 1, 2).astype(jnp.float32)
    kh = jnp.swapaxes(k, 1, 2).astype(jnp.float32)
    vh = jnp.swapaxes(v, 1, 2).astype(jnp.float32)
    scale = SB_HEAD_DIM ** -0.5
    outs = []
    for blk in range(T // SB_BLOCK):
        start = blk * SB_BLOCK
        stop = start + SB_BLOCK
        z = jnp.einsum('bhqd,bhkd->bhqk', qh[:, :, start:stop], kh[:, :, :stop]) * scale
        t_pos = start + jnp.arange(SB_BLOCK)[:, None]
        s_pos = jnp.arange(stop)[None, :]
        before = s_pos < t_pos
        log_keep = jnp.where(before, jax.nn.log_sigmoid(-z), 0.0)
        log_between = lax.cumsum(log_keep, axis=3, reverse=True) - log_keep
        log_w = jnp.where(before, jax.nn.log_sigmoid(z) + log_between, 0.0)
        weight = jnp.where(before, jnp.exp(log_w), 0.0)
        outs.append(jnp.einsum('bhqk,bhkd->bhqd', weight, vh[:, :, :stop]))
    o = jnp.concatenate(outs, axis=2)
    return jnp.swapaxes(o, 1, 2)


def hgrn2_chunked(q, k, v, log_f):
    B, H, T, K = q.shape
    V = v.shape[-1]
    n_chunks = T // HG_CHUNK

    def chunks(a):
        return jnp.moveaxis(a.reshape(B, H, n_chunks, HG_CHUNK, a.shape[-1]), 2, 0)

    causal = jnp.tril(jnp.ones((HG_CHUNK, HG_CHUNK), dtype=bool))[:, :, None]

    def step(state, inp):
        qc, kc, vc, gc = inp
        b = jnp.cumsum(gc, axis=2)
        b_last = b[:, :, -1:, :]
        o_inter = jnp.einsum('bhtk,bhkv->bhtv', qc * jnp.exp(b), state)
        diff = b[:, :, :, None, :] - b[:, :, None, :, :]
        decay = jnp.where(causal, jnp.exp(jnp.where(causal, diff, 0.0)), 0.0)
        scores = jnp.einsum('bhtk,bhsk,bhtsk->bhts', qc, kc, decay)
        o_intra = jnp.einsum('bhts,bhsv->bhtv', scores, vc)
        state = (jnp.exp(b_last[:, :, 0, :])[..., None] * state
                 + jnp.einsum('bhsk,bhsv->bhkv', kc * jnp.exp(b_last - b), vc))
        return state, o_inter + o_intra

    state0 = jnp.zeros((B, H, K, V), jnp.float32)
    _, o = lax.scan(step, state0, (chunks(q), chunks(k), chunks(v), chunks(log_f)))
    return jnp.moveaxis(o, 0, 2).reshape(B, H, T, V)


def hgrn2_mix(hq, hf, hi, hg, lb, norm_g):
    B, T, _ = hq.shape

    def heads(a):
        return jnp.swapaxes(a.reshape(B, T, HG_HEADS, -1), 1, 2).astype(jnp.float32)

    fr = hf.astype(jnp.float32)
    f = lb + (1.0 - lb) * jax.nn.sigmoid(fr)
    log_f = jnp.log(jnp.maximum(f, HG_MIN_F))
    key = (1.0 - lb) * jax.nn.sigmoid(-fr)
    o = hgrn2_chunked(heads(hq), heads(key), heads(hi), heads(log_f))
    o = jnp.swapaxes(o, 1, 2)
    o = o * lax.rsqrt(jnp.mean(o * o, axis=-1, keepdims=True) + NORM_EPS) * norm_g
    return o.reshape(B, T, HG_WIDTH) * jax.nn.silu(hg.astype(jnp.float32))


def rwkv7_recurrence(r, decay, k, v, kk, a):
    B, T, H, N = r.shape

    def step(state, inp):
        r_t, w_t, k_t, v_t, kk_t, a_t = inp
        sa = jnp.einsum('bhvk,bhk->bhv', state, -kk_t)
        state = (state * w_t[:, :, None, :]
                 + sa[..., None] * (kk_t * a_t)[:, :, None, :]
                 + v_t[..., None] * k_t[:, :, None, :])
        return state, jnp.einsum('bhvk,bhk->bhv', state, r_t)

    xs = tuple(jnp.moveaxis(t, 1, 0) for t in (r, decay, k, v, kk, a))
    _, y = lax.scan(step, jnp.zeros((B, H, N, N), jnp.float32), xs)
    return jnp.moveaxis(y, 0, 1)


def rwkv7_mix(rw, mu, w0, w_w2, a0, w_a2, w_g2, k_k, k_a, r_k, lnx_w, lnx_b):
    B, T, _ = rw.shape
    rwf = rw.astype(jnp.float32)
    prev = jnp.pad(rwf, ((0, 0), (1, 0), (0, 0)))[:, :-1]
    rwf = rwf + mu * (prev - rwf)
    r, k, v, w_in, a_in, g_in = jnp.split(rwf, RW_SPLITS, axis=-1)
    w_log = -jax.nn.softplus(-(w0 + jnp.tanh(w_in) @ w_w2)) - 0.5
    decay = jnp.exp(-jnp.exp(w_log))
    a = jax.nn.sigmoid(a0 + a_in @ w_a2)
    g = jax.nn.sigmoid(g_in) @ w_g2

    def heads(t):
        return t.reshape(B, T, RW_HEADS, RW_HEAD_DIM)

    kk = heads(k * k_k)
    kk = kk * lax.rsqrt(jnp.maximum(jnp.sum(kk * kk, axis=-1, keepdims=True), 1e-24))
    k = k * (1.0 + (a - 1.0) * k_a)
    rh, kh, vh = heads(r), heads(k), heads(v)
    y = rwkv7_recurrence(rh, heads(decay), kh, vh, kk, heads(a))
    mean = jnp.mean(y, axis=-1, keepdims=True)
    var = jnp.mean(jnp.square(y - mean), axis=-1, keepdims=True)
    y = ((y - mean) * lax.rsqrt(var + RW_GN_EPS)).reshape(B, T, RW_WIDTH) * lnx_w + lnx_b
    bonus = jnp.sum(rh * kh * r_k, axis=-1, keepdims=True) * vh
    return (y + bonus.reshape(B, T, RW_WIDTH)) * g


def setup_inputs(seed: int = 0) -> dict:
    key = jax.random.key(seed)
    ks = jax.random.split(key, 24)
    f32 = jnp.float32
    nrm = lambda k, shape: jax.random.normal(k, shape, f32)
    return {
        "x": nrm(ks[0], (BATCH, SEQ, D_MODEL)),
        "norm1_g": 1.0 + 0.02 * nrm(ks[1], (DEPTH, D_MODEL)),
        "w_in": nrm(ks[2], (DEPTH, D_MODEL, IN_COLS)) * D_MODEL ** -0.5,
        "sb_norm_g": 1.0 + 0.02 * nrm(ks[3], (DEPTH, SB_WIDTH)),
        "hg_lb_param": 0.1 * nrm(ks[4], (DEPTH, HG_WIDTH)),
        "hg_norm_g": 1.0 + 0.02 * nrm(ks[5], (DEPTH, HG_HEAD_V)),
        "rw_mu": jax.random.uniform(ks[6], (DEPTH, RW_COLS), f32, 0.1, 0.9),
        "rw_w0": jax.random.uniform(ks[7], (DEPTH, RW_WIDTH), f32, -5.0, 0.0),
        "rw_w_w2": nrm(ks[8], (DEPTH, RW_DECAY_RANK, RW_WIDTH)) * RW_DECAY_RANK ** -0.5,
        "rw_a0": 0.1 * nrm(ks[9], (DEPTH, RW_WIDTH)),
        "rw_w_a2": nrm(ks[10], (DEPTH, RW_AAA_RANK, RW_WIDTH)) * RW_AAA_RANK ** -0.5,
        "rw_w_g2": nrm(ks[11], (DEPTH, RW_GATE_RANK, RW_WIDTH)) * RW_GATE_RANK ** -0.5,
        "rw_k_k": 0.85 + 0.05 * nrm(ks[12], (DEPTH, RW_WIDTH)),
        "rw_k_a": 1.0 + 0.05 * nrm(ks[13], (DEPTH, RW_WIDTH)),
        "rw_r_k": 0.1 * nrm(ks[14], (DEPTH, RW_HEADS, RW_HEAD_DIM)),
        "rw_lnx_w": 1.0 + 0.02 * nrm(ks[15], (DEPTH, RW_WIDTH)),
        "rw_lnx_b": 0.02 * nrm(ks[16], (DEPTH, RW_WIDTH)),
        "w_out": nrm(ks[17], (DEPTH, MIX_WIDTH, D_MODEL)) * MIX_WIDTH ** -0.5,
        "norm2_g": 1.0 + 0.02 * nrm(ks[18], (DEPTH, D_MODEL)),
        "w_ff_in": nrm(ks[19], (DEPTH, D_MODEL, D_FF)) * D_MODEL ** -0.5,
        "w_ff_out": nrm(ks[20], (DEPTH, D_FF, D_MODEL)) * D_FF ** -0.5,
        "final_g": 1.0 + 0.02 * nrm(ks[21], (D_MODEL,)),
    }


def reference(x, norm1_g, w_in, sb_norm_g, hg_lb_param, hg_norm_g, rw_mu, rw_w0, rw_w_w2,
              rw_a0, rw_w_a2, rw_w_g2, rw_k_k, rw_k_a, rw_r_k, rw_lnx_w, rw_lnx_b,
              w_out, norm2_g, w_ff_in, w_ff_out, final_g):
    B, T, _ = x.shape
    probs = jax.nn.softmax(hg_lb_param.astype(jnp.float32), axis=0)
    lower_bounds = jnp.cumsum(probs, axis=0) - probs[0:1]
    for l in range(DEPTH):
        h = rms_norm(x, norm1_g[l])
        proj = h @ w_in[l]
        sb_q, sb_k, sb_v, hg_q, hg_f, hg_i, hg_g, rw = jnp.split(proj, IN_SPLITS, axis=-1)
        o_sb = stick_breaking_attention(sb_q.reshape(B, T, SB_HEADS, SB_HEAD_DIM),
                                        sb_k.reshape(B, T, SB_HEADS, SB_HEAD_DIM),
                                        sb_v.reshape(B, T, SB_HEADS, SB_HEAD_DIM))
        o_sb = o_sb * lax.rsqrt(jnp.mean(o_sb * o_sb, axis=-1, keepdims=True) + NORM_EPS)
        o_sb = o_sb.reshape(B, T, SB_WIDTH) * sb_norm_g[l]
        o_hg = hgrn2_mix(hg_q, hg_f, hg_i, hg_g, lower_bounds[l], hg_norm_g[l])
        o_rw = rwkv7_mix(rw, rw_mu[l], rw_w0[l], rw_w_w2[l], rw_a0[l], rw_w_a2[l], rw_w_g2[l],
                         rw_k_k[l], rw_k_a[l], rw_r_k[l], rw_lnx_w[l], rw_lnx_b[l])
        mix = jnp.concatenate([o_sb, o_hg, o_rw], axis=-1).astype(x.dtype)
        x = x + mix @ w_out[l]
        h = rms_norm(x, norm2_g[l])
        x = x + jnp.square(jax.nn.relu(h @ w_ff_in[l])) @ w_ff_out[l]
    return rms_norm(x, final_g)
```

```python
import numpy as np
import concourse.bass as bass
import concourse.mybir as mybir
from concourse.bass_utils import run_bass_kernel_spmd
from contextlib import ExitStack

F32 = mybir.dt.float32
BF16 = mybir.dt.bfloat16
AF = mybir.ActivationFunctionType
ALU = mybir.AluOpType
AX = mybir.AxisListType

NDSEM = 24


class Buf:
    __slots__ = ("t", "w", "r", "name", "psum")

    def __init__(self, t, name, psum=False):
        self.psum = psum
        self.t = t
        self.w = {}
        self.r = {}
        self.name = name

    def __getitem__(self, key):
        return self.t[key]


class KB:
    def __init__(self, nc, es):
        self.nc = nc
        self.es = es
        self.eng = {"pe": nc.tensor, "act": nc.scalar, "dve": nc.vector, "pool": nc.gpsimd, "sp": nc.sync}
        self.sem = {}
        self.cnt = {}
        for e in self.eng:
            self.sem[e] = es.enter_context(nc.semaphore("s_" + e))
            self.cnt[e] = 0
        self.dsems = []
        for i in range(NDSEM):
            nm = "d%d" % i
            self.sem[nm] = es.enter_context(nc.semaphore("s_" + nm))
            self.cnt[nm] = 0
            self.dsems.append(nm)
        self.dnext = 0
        self.seen = {e: {} for e in self.eng}
        self.nid = 0
        self.ninst = 0
        self.mute = False

    def sb(self, shape, dtype, name=None, es=None):
        self.nid += 1
        name = "%s_%d" % (name or "sb", self.nid)
        t = (es or self.es).enter_context(self.nc.sbuf_tensor(name, list(shape), dtype))
        return Buf(t, name)

    def ps(self, shape, dtype=F32, name=None, es=None):
        self.nid += 1
        name = "%s_%d" % (name or "ps", self.nid)
        t = (es or self.es).enter_context(self.nc.psum_tensor(name, list(shape), dtype))
        return Buf(t, name, psum=True)

    def dram(self, name, shape, dtype, kind="Internal"):
        t = self.nc.dram_tensor(name, list(shape), dtype, kind=kind).ap()
        return Buf(t, name)

    def _deps(self, r, w, e=None):
        d = {}
        for b in r:
            for s, v in b.w.items():
                if d.get(s, 0) < v:
                    d[s] = v
            if b.psum:
                for s, v in b.r.items():
                    if s != e and d.get(s, 0) < v:
                        d[s] = v
        for b in w:
            for s, v in b.w.items():
                if d.get(s, 0) < v:
                    d[s] = v
            for s, v in b.r.items():
                if d.get(s, 0) < v:
                    d[s] = v
        return d

    def _wait(self, e, d):
        seen = self.seen[e]
        en = self.eng[e]
        for s, v in d.items():
            if s == e and e == "pe":
                continue
            if seen.get(s, 0) < v:
                en.wait_ge(self.sem[s], v)
                seen[s] = v
                self.ninst += 1

    def op(self, e, fn, r=(), w=()):
        if self.mute:
            return None
        d = self._deps(r, w, e)
        self._wait(e, d)
        ins = fn(self.eng[e])
        self.cnt[e] += 1
        ins.then_inc(self.sem[e], 1)
        v = self.cnt[e]
        for b in r:
            b.r[e] = v
        for b in w:
            b.w[e] = v
        self.ninst += 1
        return ins

    def dma(self, q, out, in_, r=(), w=(), **kw):
        if self.mute:
            return None
        d = self._deps(r, w)
        ds = self.dsems[self.dnext]
        self.dnext = (self.dnext + 1) % NDSEM
        if self.cnt[ds] > 0:
            d[ds] = max(d.get(ds, 0), self.cnt[ds])
        self._wait(q, d)
        ins = self.eng[q].dma_start(out=out, in_=in_, **kw)
        self.cnt[ds] += 16
        ins.then_inc(self.sem[ds], 16)
        v = self.cnt[ds]
        for b in r:
            b.r[ds] = v
        for b in w:
            b.w[ds] = v
        self.ninst += 1
        return ins

    def barrier(self, engines=None):
        d = {s: v for s, v in self.cnt.items() if v > 0}
        for e in (engines or self.eng):
            self._wait(e, dict(d))

    def finish(self, outs):
        d = self._deps(outs, [])
        self._wait("sp", d)
        self.barrier(["sp"])

D = 4096
DFF = 16384
KC = D // 128
EPS = 1e-5


class Stream:
    def __init__(self, k, bufs, loads, ahead=None):
        self.k, self.bufs, self.loads = k, bufs, loads
        self.issued = 0
        self.n = len(bufs)

    def get(self, i):
        upto = i + self.n - 1
        while self.issued <= upto and self.issued < len(self.loads):
            self.loads[self.issued](self.bufs[self.issued % self.n])
            self.issued += 1
        return self.bufs[i % self.n]


def make_masks(k, es):
    c = {}
    ones = k.sb([128, 128], F32, "ones", es)
    k.op("pool", lambda e: e.memset(ones[:], 1.0), w=[ones])
    idf = k.sb([128, 128], F32, "idf", es)
    k.op("pool", lambda e: e.affine_select(out=idf[:], in_=ones[:], pattern=[[-1, 128]],
                                           compare_op=ALU.is_equal, fill=0.0, base=0,
                                           channel_multiplier=1), r=[ones], w=[idf])
    idb = k.sb([128, 128], BF16, "idb", es)
    k.op("pool", lambda e: e.tensor_copy(out=idb[:], in_=idf[:]), r=[idf], w=[idb])
    c["ones"], c["idf"], c["idb"] = ones, idf, idb
    return c


_rr = [0]


def cast_any(k, out_ap, in_ap, r, w, scale=None, engines=("act", "dve", "pool")):
    e = engines[_rr[0] % len(engines)]
    _rr[0] += 1
    if e == "act":
        if scale is None:
            k.op("act", lambda en: en.activation(out=out_ap, in_=in_ap, func=AF.Copy), r=r, w=w)
        else:
            k.op("act", lambda en: en.activation(out=out_ap, in_=in_ap, func=AF.Copy, scale=scale), r=r, w=w)
    else:
        if scale is None:
            k.op(e, lambda en: en.tensor_copy(out=out_ap, in_=in_ap), r=r, w=w)
        else:
            k.op(e, lambda en: en.tensor_scalar(out=out_ap, in0=in_ap, scalar1=scale, scalar2=None,
                                                op0=ALU.mult), r=r, w=w)


def prep_weight(k, src, R, C, dst, dst_view, scale_cols=None, scale_buf=None, CW=4096):
    with ExitStack() as es:
        st = [k.sb([128, CW], F32, "wst", es) for _ in range(2)]
        sb = [k.sb([128, CW], BF16, "wsb", es) for _ in range(2)]
        items = [(rc, c0) for rc in range(R // 128) for c0 in range(0, C, CW)]

        def load(i):
            rc, c0 = items[i]
            cw = min(CW, C - c0)
            a = st[i % 2]
            k.dma("sp", a[:, :cw], src[rc * 128:(rc + 1) * 128, c0:c0 + cw], w=[a])

        load(0)
        for i, (rc, c0) in enumerate(items):
            if i + 1 < len(items):
                load(i + 1)
            cw = min(CW, C - c0)
            a, b = st[i % 2], sb[i % 2]
            sc = None if scale_cols is None else scale_cols[:, rc:rc + 1]
            rr = [a] if scale_buf is None else [a, scale_buf]
            cast_any(k, b[:, :cw], a[:, :cw], rr, [b], scale=sc)
            dap, sview = dst_view(rc, c0, cw)
            k.dma("sp", dap, sview(b[:, :cw]), r=[b], w=[dst])
        k.barrier()


def rms_rstd(k, x, junk, ss, rstd, small_r=()):
    k.op("act", lambda e: e.activation(out=junk[:], in_=x[:], func=AF.Square, accum_out=ss[:, 0:1]),
         r=[x], w=[junk, ss])
    k.op("dve", lambda e: e.tensor_scalar(out=ss[:, 1:2], in0=ss[:, 0:1], scalar1=1.0 / D, scalar2=EPS,
                                          op0=ALU.mult, op1=ALU.add), r=[ss], w=[ss])
    k.op("act", lambda e: e.activation(out=ss[:, 2:3], in_=ss[:, 1:2], func=AF.Sqrt), r=[ss], w=[ss])
    k.op("dve", lambda e: e.reciprocal(out=rstd[:, 0:1], in_=ss[:, 2:3]), r=[ss], w=[rstd])


def transpose_rows(k, src_bf, dstT, tok_off, idb, tps, tcount):
    for g in range(KC // 8):
        tp = tps[tcount[0] % len(tps)]
        tcount[0] += 1
        for j in range(8):
            kc = g * 8 + j
            k.op("pe", lambda e: e.transpose(out=tp[:, j, :], in_=src_bf[:, kc * 128:(kc + 1) * 128],
                                             identity=idb[:]), r=[src_bf, idb], w=[tp])
        eng = "act" if (tcount[0] % 2) else "dve"
        if eng == "act":
            k.op("act", lambda e: e.activation(out=dstT[:, g * 8:(g + 1) * 8, tok_off:tok_off + 128],
                                               in_=tp[:, :, :], func=AF.Copy), r=[tp], w=[dstT])
        else:
            k.op("dve", lambda e: e.tensor_copy(out=dstT[:, g * 8:(g + 1) * 8, tok_off:tok_off + 128],
                                                in_=tp[:, :, :]), r=[tp], w=[dstT])


def build_B(nc, NT, last, dff=DFF):
    TG = 256
    NG = NT // TG
    NFP = dff // 256
    NFC = dff // 128
    NPC = NFC // 16
    x_in = nc.dram_tensor("x", [NT, D], F32, kind="ExternalInput").ap()
    mix_in = nc.dram_tensor("mixT", [D, NT], F32, kind="ExternalInput").ap()
    wo_in = nc.dram_tensor("w_out", [D, D], F32, kind="ExternalInput").ap()
    g2_in = nc.dram_tensor("norm2_g", [D], F32, kind="ExternalInput").ap()
    w1_in = nc.dram_tensor("w_ff_in", [D, dff], F32, kind="ExternalInput").ap()
    w2_in = nc.dram_tensor("w_ff_out", [dff, D], F32, kind="ExternalInput").ap()
    gf_in = nc.dram_tensor("final_g", [D], F32, kind="ExternalInput").ap()
    y_out = nc.dram_tensor("y", [NT, D], F32, kind="ExternalOutput").ap()
    with ExitStack() as es:
        k = KB(nc, es)
        yb = Buf(y_out, "y")
        wo_s = k.dram("wo_s", [D // 256, 128, KC, 256], BF16)
        w1_s = k.dram("w1_s", [NFP, 128, KC, 256], BF16)
        w2_s = k.dram("w2_s", [D // 512, NPC, 128, 16, 512], BF16)
        cm = make_masks(k, es)
        g2c = k.sb([128, KC], F32, "g2c", es)
        k.dma("sp", g2c[:, :], g2_in.rearrange("(c p) -> p c", p=128), w=[g2c], allow_slow_non_contiguous=True)

        def v_kcp(dst):
            def f(rc, c0, cw):
                n = cw // 256
                p0 = c0 // 256
                return (dst.t[p0:p0 + n, :, rc, :].rearrange("n p c -> p n c"),
                        lambda a: a.rearrange("p (n c) -> p n c", c=256))
            return f

        def v_w2(rc, c0, cw):
            pc, j = rc // 16, rc % 16
            n = cw // 512
            d0 = c0 // 512
            return (w2_s.t[d0:d0 + n, pc, :, j, :].rearrange("n p c -> p n c"),
                    lambda a: a.rearrange("p (n c) -> p n c", c=512))

        prep_weight(k, wo_in, D, D, wo_s, v_kcp(wo_s))
        prep_weight(k, w1_in, D, dff, w1_s, v_kcp(w1_s), scale_cols=g2c, scale_buf=g2c)
        prep_weight(k, w2_in, dff, D, w2_s, v_w2)

        xt = [k.sb([128, D], F32, "xt", es) for _ in range(2)]
        mst = k.sb([128, D], F32, "mst", es)
        hb = k.sb([128, D], BF16, "hb", es)
        aT = k.sb([128, KC, TG], BF16, "aT", es)
        hid = k.sb([128, NFC, TG], BF16, "hid", es)
        wr = [k.sb([128, 8192], BF16, "wr", es) for _ in range(3)]
        ss = [k.sb([128, 4], F32, "ss", es) for _ in range(2)]
        rstd = [k.sb([128, 1], F32, "rstd", es) for _ in range(2)]
        rl = [k.sb([128, TG], F32, "rl", es) for _ in range(2)]
        tps = [k.ps([128, 8, 128], BF16, "tp", es) for _ in range(2)]
        mmp = [k.ps([128, 512], F32, "mm", es) for _ in range(4)]
        tcount = [0]

        loads = []
        for g in range(NG):
            for p in range(D // 256):
                loads.append(lambda b, p=p: k.dma("sp", b[:, :], wo_s.t[p].rearrange("p k c -> p (k c)"),
                                                  r=[wo_s], w=[b]))
            for p in range(NFP):
                loads.append(lambda b, p=p: k.dma("sp", b[:, :], w1_s.t[p].rearrange("p k c -> p (k c)"),
                                                  r=[w1_s], w=[b]))
            for db in range(D // 512):
                for pc in range(NPC):
                    loads.append(lambda b, db=db, pc=pc: k.dma(
                        "sp", b[:, :], w2_s.t[db, pc].rearrange("p k c -> p (k c)"), r=[w2_s], w=[b]))
        stream = Stream(k, wr, loads)
        wi = 0
        if last:
            gft = k.sb([128, D], F32, "gft", es)
            k.dma("sp", gft[:, :], gf_in.partition_broadcast(128), w=[gft])

        for g in range(NG):
            t0 = g * TG
            for ti in range(2):
                r0 = t0 + ti * 128
                k.dma("act", xt[ti][:, :], x_in[r0:r0 + 128, :], w=[xt[ti]])
            mixv = mix_in.rearrange("(k p) t -> p k t", p=128)
            for half in range(2):
                k.dma("act", mst[:, :].rearrange("p (k t) -> p k t", t=TG),
                      mixv[:, half * 16:(half + 1) * 16, t0:t0 + TG], w=[mst])
                cast_any(k, aT[:, half * 16:(half + 1) * 16, :], mst[:, :].rearrange("p (k t) -> p k t", t=TG),
                         [mst], [aT], engines=("act", "dve"))
            mi = 0
            for p in range(D // 256):
                wb = stream.get(wi)
                wi += 1
                wv = wb[:, :].rearrange("p (k c) -> p k c", c=256)
                for ti in range(2):
                    ps = mmp[mi % 4]
                    mi += 1
                    for kc in range(KC):
                        k.op("pe", lambda e: e.matmul(ps[:, 0:256], lhsT=aT[:, kc, ti * 128:(ti + 1) * 128],
                                                      rhs=wv[:, kc, :], start=(kc == 0), stop=(kc == KC - 1)),
                             r=[aT, wb], w=[ps])
                    k.op("dve", lambda e: e.tensor_tensor(out=xt[ti][:, p * 256:(p + 1) * 256],
                                                          in0=xt[ti][:, p * 256:(p + 1) * 256],
                                                          in1=ps[:, 0:256], op=ALU.add), r=[ps, xt[ti]], w=[xt[ti]])
            for ti in range(2):
                rms_rstd(k, xt[ti], hb, ss[ti], rstd[ti])
                k.op("act", lambda e: e.activation(out=hb[:, :], in_=xt[ti][:, :], func=AF.Copy,
                                                   scale=rstd[ti][:, 0:1]), r=[xt[ti], rstd[ti]], w=[hb])
                transpose_rows(k, hb, aT, ti * 128, cm["idb"], tps, tcount)
            for p in range(NFP):
                wb = stream.get(wi)
                wi += 1
                wv = wb[:, :].rearrange("p (k c) -> p k c", c=256)
                for j in range(2):
                    fc = p * 2 + j
                    ps = mmp[mi % 4]
                    mi += 1
                    for kc in range(KC):
                        k.op("pe", lambda e: e.matmul(ps[:, 0:TG], lhsT=wv[:, kc, j * 128:(j + 1) * 128],
                                                      rhs=aT[:, kc, :], start=(kc == 0), stop=(kc == KC - 1)),
                             r=[aT, wb], w=[ps])
                    r_ = rl[fc % 2]
                    k.op("act", lambda e: e.activation(out=r_[:, :], in_=ps[:, 0:TG], func=AF.Relu),
                         r=[ps], w=[r_])
                    eng = "dve" if fc % 2 == 0 else "pool"
                    k.op(eng, lambda e: e.tensor_tensor(out=hid[:, fc, :], in0=r_[:, :], in1=r_[:, :],
                                                        op=ALU.mult), r=[r_], w=[hid])
            for db in range(D // 512):
                pss = [mmp[(mi + ti) % 4] for ti in range(2)]
                mi += 2
                for pc in range(NPC):
                    wb = stream.get(wi)
                    wi += 1
                    wv = wb[:, :].rearrange("p (k c) -> p k c", c=512)
                    for ti in range(2):
                        for j in range(16):
                            fc = pc * 16 + j
                            k.op("pe", lambda e: e.matmul(pss[ti][:, :], lhsT=hid[:, fc, ti * 128:(ti + 1) * 128],
                                                          rhs=wv[:, j, :], start=(fc == 0), stop=(fc == NFC - 1)),
                                 r=[hid, wb], w=[pss[ti]])
                for ti in range(2):
                    k.op("dve", lambda e: e.tensor_tensor(out=xt[ti][:, db * 512:(db + 1) * 512],
                                                          in0=xt[ti][:, db * 512:(db + 1) * 512],
                                                          in1=pss[ti][:, :], op=ALU.add),
                         r=[pss[ti], xt[ti]], w=[xt[ti]])
            for ti in range(2):
                r0 = t0 + ti * 128
                if last:
                    rms_rstd(k, xt[ti], hb, ss[ti], rstd[ti])
                    k.op("dve", lambda e: e.scalar_tensor_tensor(out=xt[ti][:, :], in0=xt[ti][:, :],
                                                                 scalar=rstd[ti][:, 0:1], in1=gft[:, :],
                                                                 op0=ALU.mult, op1=ALU.mult),
                         r=[xt[ti], rstd[ti], gft], w=[xt[ti]])
                k.dma("act", y_out[r0:r0 + 128, :], xt[ti][:, :], r=[xt[ti]], w=[yb])
        k.finish([yb])
        print("B instructions:", k.ninst)
    return nc

NCOL = 6912
NPIECE = NCOL // 256
O_SBQ, O_SBK, O_SBV = 0, 512, 1024
O_HGQ, O_HGF, O_HGI, O_HGG = 1536, 2048, 2560, 3072
O_RWR, O_RWK, O_RWV = 0, 1024, 2048
O_RWW, O_RWA, O_RWG = 3072, 3168, 3264
RW_ROW0 = 3584


def evac(k, i, out_ap, in_ap, r, w, engines=("act", "dve")):
    e = engines[i % len(engines)]
    if e == "act":
        k.op("act", lambda en: en.activation(out=out_ap, in_=in_ap, func=AF.Copy), r=r, w=w)
    else:
        k.op(e, lambda en: en.tensor_copy(out=out_ap, in_=in_ap), r=r, w=w)


def phase_inproj(k, cm, x_in, T, wi_s, projT, projR):
    TGA = min(T, 1024)
    NG = T // TGA
    with ExitStack() as es:
        hT = k.sb([128, KC, TGA], BF16, "hT", es)
        xt = [k.sb([128, D], F32, "xt", es) for _ in range(2)]
        hb = k.sb([128, D], BF16, "hb", es)
        ss = [k.sb([128, 4], F32, "ss", es) for _ in range(2)]
        rstd = [k.sb([128, 1], F32, "rstd", es) for _ in range(2)]
        wr = [k.sb([128, 8192], BF16, "wr", es) for _ in range(3)]
        ost = [k.sb([128, 512], F32, "ost", es) for _ in range(4)]
        tps = [k.ps([128, 8, 128], BF16, "tp", es) for _ in range(2)]
        mmp = [k.ps([128, 512], F32, "mm", es) for _ in range(4)]
        tcount = [0]
        loads = []
        for g in range(NG):
            for p in range(NPIECE):
                loads.append(lambda b, p=p: k.dma("sp", b[:, :], wi_s.t[p].rearrange("p k c -> p (k c)"),
                                                  r=[wi_s], w=[b]))
        stream = Stream(k, wr, loads)
        wi = 0
        mi = 0
        for g in range(NG):
            t0 = g * TGA
            for ti in range(TGA // 128):
                x = xt[ti % 2]
                r0 = t0 + ti * 128
                k.dma("act", x[:, :], x_in[r0:r0 + 128, :], w=[x])
                rms_rstd(k, x, hb, ss[ti % 2], rstd[ti % 2])
                k.op("act", lambda e: e.activation(out=hb[:, :], in_=x[:, :], func=AF.Copy,
                                                   scale=rstd[ti % 2][:, 0:1]), r=[x, rstd[ti % 2]], w=[hb])
                transpose_rows(k, hb, hT, ti * 128, cm["idb"], tps, tcount)
            for p in range(NPIECE):
                wb = stream.get(wi)
                wi += 1
                wv = wb[:, :].rearrange("p (k c) -> p k c", c=256)
                for j in range(2):
                    for tc in range(TGA // 512):
                        ps = mmp[mi % 4]
                        o = ost[mi % 4]
                        for kc in range(KC):
                            k.op("pe", lambda e: e.matmul(ps[:, :], lhsT=wv[:, kc, j * 128:(j + 1) * 128],
                                                          rhs=hT[:, kc, tc * 512:(tc + 1) * 512],
                                                          start=(kc == 0), stop=(kc == KC - 1)),
                                 r=[hT, wb], w=[ps])
                        evac(k, mi, o[:, :], ps[:, :], [ps], [o])
                        mi += 1
                        row = (p * 2 + j) * 128
                        pd, prow = (projT, row) if row < RW_ROW0 else (projR, row - RW_ROW0)
                        k.dma("pool", pd.t[prow:prow + 128, t0 + tc * 512:t0 + (tc + 1) * 512], o[:, :],
                              r=[o], w=[pd])
        k.barrier()


def mk_mask(k, es, shape, pattern, base, cmult, op, name):
    n = 1
    for s in shape[1:]:
        n *= s
    one = k.sb(shape, F32, name + "1", es)
    m = k.sb(shape, F32, name, es)
    k.op("pool", lambda e: e.memset(one[:], 1.0), w=[one])
    k.op("pool", lambda e: e.affine_select(out=m[:], in_=one[:], pattern=pattern, compare_op=op, fill=0.0,
                                           base=base, channel_multiplier=cmult), r=[one], w=[m])
    return m


def phase_sb(k, cm, projT, T, sbg_in, mixT, row0):
    NQC = T // 512
    scale = 128.0 ** -0.5
    with ExitStack() as es:
        uge = mk_mask(k, es, [128, 128], [[-1, 128]], 0, 1, ALU.is_ge, "uge")
        ones = cm["ones"]
        dmask = [mk_mask(k, es, [128, 512], [[1, 512]], -128 * jj, -1, ALU.is_gt, "dm%d" % jj) for jj in range(4)]
        gcol = k.sb([128, 4], F32, "gcol", es)
        k.dma("sp", gcol[:, :], sbg_in.rearrange("(h p) -> p h", p=128), w=[gcol], allow_slow_non_contiguous=True)
        one1 = k.sb([128, 1], F32, "one1", es)
        k.op("pool", lambda e: e.memset(one1[:], 1.0), w=[one1])
        st = [k.sb([128, T], F32, "st", es) for _ in range(2)]
        q_bf = k.sb([128, T], BF16, "q_bf", es)
        k_bf = k.sb([128, T], BF16, "k_bf", es)
        v_bf = k.sb([128, T], BF16, "v_bf", es)
        v_tok = k.sb([128, T // 128, 128], BF16, "v_tok", es)
        Rb = k.sb([128, 512], F32, "Rb", es)
        E = [k.sb([128, 512], F32, "E", es) for _ in range(2)]
        Lp = [k.sb([128, 512], F32, "Lp", es) for _ in range(2)]
        t1 = [k.sb([128, 512], F32, "t1", es) for _ in range(2)]
        lw = [k.sb([128, 512], F32, "lw", es) for _ in range(2)]
        W = [k.sb([128, 512], BF16, "W", es) for _ in range(2)]
        sq = k.sb([128, 512], F32, "sq", es)
        rs = k.sb([128, 512], F32, "rs", es)
        ob = [k.sb([128, 512], F32, "ob", es) for _ in range(2)]
        tp = k.ps([128, 8, 128], BF16, "tp", es)
        zp = [k.ps([128, 512], F32, "zp", es) for _ in range(2)]
        qp = [k.ps([128, 512], F32, "qp", es) for _ in range(2)]
        cp = k.ps([128, 512], F32, "cp", es)
        op_ = [k.ps([128, 512], F32, "op", es) for _ in range(2)]
        si = 0
        for h in range(4):
            for (off, dst, n) in ((O_SBQ, q_bf, 0), (O_SBK, k_bf, 1), (O_SBV, v_bf, 2)):
                s_ = st[n % 2]
                k.dma("sp", s_[:, :], projT.t[off + h * 128:off + (h + 1) * 128, :], r=[projT], w=[s_])
                cast_any(k, dst[:, :], s_[:, :], [s_], [dst], engines=("act", "dve"))
            for g in range(T // 1024 if T >= 1024 else 1):
                nb = min(8, T // 128)
                for j in range(nb):
                    kb = g * 8 + j
                    k.op("pe", lambda e: e.transpose(out=tp[:, j, :], in_=v_bf[:, kb * 128:(kb + 1) * 128],
                                                     identity=cm["idb"][:]), r=[v_bf, cm["idb"]], w=[tp])
                k.op("dve", lambda e: e.tensor_copy(out=v_tok[:, g * 8:g * 8 + nb, :], in_=tp[:, 0:nb, :]),
                     r=[tp], w=[v_tok])
            for c in range(NQC):
                ops = op_[c % 2]
                first = True
                for kb in range(4 * c + 3, -1, -1):
                    i = si % 2
                    si += 1
                    z = zp[i]
                    k.op("pe", lambda e: e.matmul(z[:, :], lhsT=k_bf[:, kb * 128:(kb + 1) * 128],
                                                  rhs=q_bf[:, c * 512:(c + 1) * 512], start=True, stop=True),
                         r=[k_bf, q_bf], w=[z])
                    k.op("act", lambda e: e.activation(out=E[i][:, :], in_=z[:, :], func=AF.Exp, scale=scale),
                         r=[z], w=[E[i]])
                    k.op("act", lambda e: e.activation(out=Lp[i][:, :], in_=E[i][:, :], func=AF.Ln,
                                                       bias=one1[:, 0:1]), r=[E[i], one1], w=[Lp[i]])
                    jj = kb - 4 * c
                    if jj >= 0:
                        k.op("pool", lambda e: e.tensor_tensor(out=Lp[i][:, :], in0=Lp[i][:, :],
                                                               in1=dmask[jj][:, :], op=ALU.mult),
                             r=[Lp[i], dmask[jj]], w=[Lp[i]])
                    k.op("pe", lambda e: e.matmul(qp[i][:, :], lhsT=uge[:, :], rhs=Lp[i][:, :], start=True, stop=True),
                         r=[uge, Lp[i]], w=[qp[i]])
                    if kb > 0:
                        k.op("pe", lambda e: e.matmul(cp[:, :], lhsT=ones[:, :], rhs=Lp[i][:, :], start=True,
                                                      stop=True), r=[ones, Lp[i]], w=[cp])
                    if first:
                        k.op("pool", lambda e: e.memset(Rb[:, :], 0.0), w=[Rb])
                    k.op("dve", lambda e: e.tensor_tensor(out=t1[i][:, :], in0=qp[i][:, :], in1=Rb[:, :],
                                                          op=ALU.add), r=[qp[i], Rb], w=[t1[i]])
                    k.op("dve", lambda e: e.scalar_tensor_tensor(out=lw[i][:, :], in0=z[:, :], scalar=scale,
                                                                 in1=t1[i][:, :], op0=ALU.mult,
                                                                 op1=ALU.subtract), r=[z, t1[i]], w=[lw[i]])
                    k.op("act", lambda e: e.activation(out=W[i][:, :], in_=lw[i][:, :], func=AF.Exp),
                         r=[lw[i]], w=[W[i]])
                    if jj >= 0:
                        k.op("pool", lambda e: e.tensor_tensor(out=W[i][:, :], in0=W[i][:, :],
                                                               in1=dmask[jj][:, :], op=ALU.mult),
                             r=[W[i], dmask[jj]], w=[W[i]])
                    if kb > 0:
                        k.op("dve", lambda e: e.tensor_tensor(out=Rb[:, :], in0=Rb[:, :], in1=cp[:, :],
                                                              op=ALU.add), r=[cp, Rb], w=[Rb])
                    k.op("pe", lambda e: e.matmul(ops[:, :], lhsT=v_tok[:, kb, :], rhs=W[i][:, :], start=first,
                                                  stop=(kb == 0)), r=[v_tok, W[i]], w=[ops])
                    first = False
                k.op("act", lambda e: e.activation(out=sq[:, :], in_=ops[:, :], func=AF.Square), r=[ops], w=[sq])
                k.op("pe", lambda e: e.matmul(cp[:, :], lhsT=ones[:, :], rhs=sq[:, :], start=True, stop=True),
                     r=[ones, sq], w=[cp])
                k.op("dve", lambda e: e.tensor_scalar(out=rs[:, :], in0=cp[:, :], scalar1=1.0 / 128, scalar2=EPS,
                                                      op0=ALU.mult, op1=ALU.add), r=[cp], w=[rs])
                k.op("act", lambda e: e.activation(out=rs[:, :], in_=rs[:, :], func=AF.Sqrt), r=[rs], w=[rs])
                k.op("dve", lambda e: e.reciprocal(out=rs[:, :], in_=rs[:, :]), r=[rs], w=[rs])
                o = ob[c % 2]
                k.op("dve", lambda e: e.scalar_tensor_tensor(out=o[:, :], in0=ops[:, :], scalar=gcol[:, h:h + 1],
                                                             in1=rs[:, :], op0=ALU.mult, op1=ALU.mult),
                     r=[ops, gcol, rs], w=[o])
                k.dma("sp", mixT.t[row0 + h * 128:row0 + (h + 1) * 128, c * 512:(c + 1) * 512], o[:, :],
                      r=[o], w=[mixT])
        k.barrier()


def build_A(nc, T, layer, phases=("sb", "hg", "rw"), dbg=False):
    x_in = nc.dram_tensor("x", [T, D], F32, kind="ExternalInput").ap()
    g1_in = nc.dram_tensor("norm1_g", [D], F32, kind="ExternalInput").ap()
    wi_in = nc.dram_tensor("w_in", [D, NCOL], F32, kind="ExternalInput").ap()
    sbg_in = nc.dram_tensor("sb_norm_g", [512], F32, kind="ExternalInput").ap()
    lbp_in = nc.dram_tensor("hg_lb_param", [2, 512], F32, kind="ExternalInput").ap()
    hgn_in = nc.dram_tensor("hg_norm_g", [128], F32, kind="ExternalInput").ap()
    RWSH = {"rw_mu": [3328], "rw_w_w2": [96, 1024], "rw_w_a2": [96, 1024], "rw_w_g2": [64, 1024]}
    P = {n: nc.dram_tensor(n, RWSH.get(n, [1024]), F32, kind="ExternalInput").ap() for n in RW_PARAMS}
    mixT_out = nc.dram_tensor("mixT", [2048, T], F32, kind="ExternalOutput").ap()
    dbg_out = nc.dram_tensor("projT_dbg", [NCOL, T], F32, kind="ExternalOutput" if dbg else "Internal").ap()
    with ExitStack() as es:
        k = KB(nc, es)
        mixT = Buf(mixT_out, "mixT")
        projT = Buf(dbg_out, "projT")
        wi_s = k.dram("wi_s", [NPIECE, 128, KC, 256], BF16)
        cm = make_masks(k, es)
        g1c = k.sb([128, KC], F32, "g1c", es)
        k.dma("sp", g1c[:, :], g1_in.rearrange("(c p) -> p c", p=128), w=[g1c], allow_slow_non_contiguous=True)

        def v_kcp(rc, c0, cw):
            n = cw // 256
            p0 = c0 // 256
            return (wi_s.t[p0:p0 + n, :, rc, :].rearrange("n p c -> p n c"),
                    lambda a: a.rearrange("p (n c) -> p n c", c=256))

        prep_weight(k, wi_in, D, NCOL, wi_s, v_kcp, scale_cols=g1c, scale_buf=g1c)
        projR = k.dram("projR", [NCOL - RW_ROW0, T], F32)
        phase_inproj(k, cm, x_in, T, wi_s, projT, projR)
        if "sb" in phases:
            phase_sb(k, cm, projT, T, sbg_in, mixT, 0)
        if "hg" in phases:
            phase_hg(k, cm, projT, T, layer, lbp_in, hgn_in, mixT, 512)
        if "rw" in phases:
            phase_rw(k, cm, projR, T, P, mixT, 1024)
        k.finish([mixT, projT] if dbg else [mixT])
        print("A instructions:", k.ninst, {e: v for e, v in k.cnt.items() if not e.startswith("d")}, max(v for e, v in k.cnt.items() if e.startswith("d")))
    return nc


def phase_hg(k, cm, projT, T, layer, lbp_in, hgn_in, mixT, row0):
    TB = min(T, 1024)
    NB = T // TB
    NTL = TB // 128
    NCH = TB // 64
    with ExitStack() as es:
        m2 = mk_mask(k, es, [128, 128], [[1, 128]], 0, -1, ALU.is_ge, "m2")
        k.op("pool", lambda e: e.memset(m2[0:64, 64:128], 0.0), w=[m2])
        rmask = k.sb([128, TB], F32, "rmask", es)
        k.op("pool", lambda e: e.memset(rmask[:, :], 1.0), w=[rmask])
        k.op("pool", lambda e: e.memset(rmask[:, :].rearrange("p (c j) -> p c j", j=64)[:, :, 0:1], 0.0), w=[rmask])
        gn = k.sb([128, 1], F32, "gn", es)
        k.dma("sp", gn[:, :], hgn_in.rearrange("(p o) -> p o", o=1), w=[gn])
        lbp = k.sb([128, 4, 2], F32, "lbp", es)
        for l_ in range(2):
            for h_ in range(4):
                k.dma("sp", lbp[:, h_, l_:l_ + 1], lbp_in[l_, h_ * 128:(h_ + 1) * 128].rearrange("(p o) -> p o", o=1), w=[lbp])
        ee = k.sb([128, 4, 2], F32, "ee", es)
        sm = k.sb([128, 4, 4], F32, "sm", es)
        lbv = k.sb([128, 4], F32, "lbv", es)
        oml = k.sb([128, 4], F32, "oml", es)
        k.op("act", lambda e: e.activation(out=ee[:, :, :], in_=lbp[:, :, :], func=AF.Exp), r=[lbp], w=[ee])
        k.op("dve", lambda e: e.tensor_tensor(out=sm[:, :, 0], in0=ee[:, :, 0], in1=ee[:, :, 1], op=ALU.add), r=[ee], w=[sm])
        k.op("dve", lambda e: e.reciprocal(out=sm[:, :, 1], in_=sm[:, :, 0]), r=[sm], w=[sm])
        k.op("dve", lambda e: e.tensor_tensor(out=sm[:, :, 2], in0=ee[:, :, 0], in1=sm[:, :, 1], op=ALU.mult), r=[ee, sm], w=[sm])
        k.op("dve", lambda e: e.tensor_tensor(out=sm[:, :, 3], in0=ee[:, :, 1], in1=sm[:, :, 1], op=ALU.mult), r=[ee, sm], w=[sm])
        if layer == 0:
            k.op("dve", lambda e: e.tensor_tensor(out=lbv[:, :], in0=sm[:, :, 2], in1=sm[:, :, 2], op=ALU.subtract), r=[sm], w=[lbv])
        else:
            k.op("dve", lambda e: e.tensor_tensor(out=sm[:, :, 0], in0=sm[:, :, 2], in1=sm[:, :, 3], op=ALU.add), r=[sm], w=[sm])
            k.op("dve", lambda e: e.tensor_tensor(out=lbv[:, :], in0=sm[:, :, 0], in1=sm[:, :, 2], op=ALU.subtract), r=[sm], w=[lbv])
        k.op("dve", lambda e: e.tensor_scalar(out=oml[:, :], in0=lbv[:, :], scalar1=-1.0, scalar2=1.0, op0=ALU.mult,
                                              op1=ALU.add), r=[lbv], w=[oml])
        S = [k.sb([128, 128], F32, "S", es) for _ in range(2)]
        names = ["qT", "fT", "gT", "vT", "A1", "K1", "Bt", "D1", "E1", "QT", "KT", "QH", "KH", "OT"]
        t_ = {n: k.sb([128, TB], F32, n, es) for n in names}
        Vt = k.sb([128, NTL, 128], F32, "Vt", es)
        KHt = k.sb([128, NTL, 128], F32, "KHt", es)
        SC = [k.sb([128, 128], F32, "SC", es) for _ in range(2)]
        EL = k.sb([128, NCH], F32, "EL", es)
        sq = k.sb([128, 512], F32, "sq", es)
        rs = k.sb([128, 512], F32, "rs", es)
        sil = k.sb([128, 512], F32, "sil", es)
        ob = [k.sb([128, 512], F32, "ob", es) for _ in range(2)]
        tp = [k.ps([128, 4, 128], F32, "tp", es) for _ in range(2)]
        scp = k.ps([128, 512], F32, "scp", es)
        ops = [k.ps([128, 512], F32, "ops", es) for _ in range(2)]
        kvp = [k.ps([128, 512], F32, "kvp", es) for _ in range(2)]
        idf = cm["idf"]
        ones = cm["ones"]
        tci = 0
        oi = 0

        def v3(b):
            return b[:, :].rearrange("p (c j) -> p c j", j=64)

        for h in range(4):
            cur = 0
            k.op("pool", lambda e: e.memset(S[0][:, :], 0.0), w=[S[0]])
            for blk in range(NB):
                t0 = blk * TB
                for (off, n) in ((O_HGQ, "qT"), (O_HGF, "fT"), (O_HGG, "gT"), (O_HGI, "vT")):
                    k.dma("sp", t_[n][:, :], projT.t[off + h * 128:off + (h + 1) * 128, t0:t0 + TB], r=[projT], w=[t_[n]])
                q, f, g, v = t_["qT"], t_["fT"], t_["gT"], t_["vT"]
                A1, K1, Bt, D1, E1 = t_["A1"], t_["K1"], t_["Bt"], t_["D1"], t_["E1"]
                QT, KT, QH, KH, OT = t_["QT"], t_["KT"], t_["QH"], t_["KH"], t_["OT"]

                def transpose_tok(src, dst):
                    nonlocal tci
                    for g4 in range(NTL // 4):
                        p = tp[tci % 2]
                        tci += 1
                        for j in range(4):
                            tt = g4 * 4 + j
                            k.op("pe", lambda e: e.transpose(out=p[:, j, :], in_=src[:, tt * 128:(tt + 1) * 128],
                                                             identity=idf[:]), r=[src, idf], w=[p])
                        evac(k, tci, dst[:, g4 * 4:(g4 + 1) * 4, :], p[:, :, :], [p], [dst])

                transpose_tok(v, Vt)
                k.op("act", lambda e: e.activation(out=A1[:, :], in_=f[:, :], func=AF.Sigmoid), r=[f], w=[A1])
                k.op("dve", lambda e: e.tensor_scalar(out=A1[:, :], in0=A1[:, :], scalar1=oml[:, h:h + 1],
                                                      scalar2=lbv[:, h:h + 1], op0=ALU.mult, op1=ALU.add),
                     r=[A1, oml, lbv], w=[A1])
                k.op("dve", lambda e: e.tensor_scalar_max(out=A1[:, :], in0=A1[:, :], scalar1=1e-30), r=[A1], w=[A1])
                k.op("act", lambda e: e.activation(out=A1[:, :], in_=A1[:, :], func=AF.Ln), r=[A1], w=[A1])
                k.op("act", lambda e: e.activation(out=K1[:, :], in_=f[:, :], func=AF.Sigmoid, scale=-1.0), r=[f], w=[K1])
                k.op("pool", lambda e: e.tensor_scalar(out=K1[:, :], in0=K1[:, :], scalar1=oml[:, h:h + 1], scalar2=None,
                                                       op0=ALU.mult), r=[K1, oml], w=[K1])
                k.op("dve", lambda e: e.tensor_tensor_scan(out=Bt[:, :], data0=rmask[:, :], data1=A1[:, :], initial=0.0,
                                                           op0=ALU.mult, op1=ALU.add), r=[rmask, A1], w=[Bt])
                b3 = v3(Bt)
                k.op("dve", lambda e: e.tensor_tensor(out=v3(D1), in0=b3, in1=b3[:, :, 32:33].to_broadcast([128, NCH, 64]),
                                                      op=ALU.subtract), r=[Bt], w=[D1])
                k.op("act", lambda e: e.activation(out=E1[:, :], in_=D1[:, :], func=AF.Exp), r=[D1], w=[E1])
                k.op("dve", lambda e: e.tensor_tensor(out=QT[:, :], in0=q[:, :], in1=E1[:, :], op=ALU.mult), r=[q, E1], w=[QT])
                k.op("act", lambda e: e.activation(out=E1[:, :], in_=D1[:, :], func=AF.Exp, scale=-1.0), r=[D1], w=[E1])
                k.op("pool", lambda e: e.tensor_tensor(out=KT[:, :], in0=K1[:, :], in1=E1[:, :], op=ALU.mult), r=[K1, E1], w=[KT])
                k.op("act", lambda e: e.activation(out=E1[:, :], in_=Bt[:, :], func=AF.Exp), r=[Bt], w=[E1])
                k.op("dve", lambda e: e.tensor_tensor(out=QH[:, :], in0=q[:, :], in1=E1[:, :], op=ALU.mult), r=[q, E1], w=[QH])
                k.op("dve", lambda e: e.tensor_tensor(out=v3(D1), in0=b3[:, :, 63:64].to_broadcast([128, NCH, 64]), in1=b3,
                                                      op=ALU.subtract), r=[Bt], w=[D1])
                k.op("act", lambda e: e.activation(out=E1[:, :], in_=D1[:, :], func=AF.Exp), r=[D1], w=[E1])
                k.op("pool", lambda e: e.tensor_tensor(out=KH[:, :], in0=K1[:, :], in1=E1[:, :], op=ALU.mult), r=[K1, E1], w=[KH])
                k.op("act", lambda e: e.activation(out=EL[:, :], in_=b3[:, :, 63], func=AF.Exp), r=[Bt], w=[EL])
                transpose_tok(KH, KHt)
                for tt in range(NTL):
                    sl = slice(tt * 128, (tt + 1) * 128)
                    k.op("pe", lambda e: e.matmul(scp[:, 0:128], lhsT=KT[:, sl], rhs=QT[:, sl], start=True, stop=True),
                         r=[KT, QT], w=[scp])
                    sc = SC[tt % 2]
                    k.op("dve", lambda e: e.tensor_tensor(out=sc[:, :], in0=scp[:, 0:128], in1=m2[:, :], op=ALU.mult),
                         r=[scp, m2], w=[sc])
                    for half in range(2):
                        cc = tt * 2 + half
                        c0 = tt * 128 + half * 64
                        op = ops[oi % 2]
                        kv = kvp[oi % 2]
                        oi += 1
                        Sc, Sn = S[cur], S[1 - cur]
                        k.op("pe", lambda e: e.matmul(op[:, 0:64], lhsT=Sc[:, :], rhs=QH[:, c0:c0 + 64], start=True,
                                                      stop=False), r=[Sc, QH], w=[op])
                        k.op("pe", lambda e: e.matmul(op[:, 0:64], lhsT=Vt[:, tt, :], rhs=sc[:, half * 64:(half + 1) * 64],
                                                      start=False, stop=True), r=[Vt, sc], w=[op])
                        pr = slice(half * 64, (half + 1) * 64)
                        k.op("pe", lambda e: e.matmul(kv[:, 0:128], lhsT=KHt[pr, tt, :], rhs=Vt[pr, tt, :], start=True,
                                                      stop=True), r=[KHt, Vt], w=[kv])
                        k.op("dve", lambda e: e.scalar_tensor_tensor(out=Sn[:, :], in0=Sc[:, :], scalar=EL[:, cc:cc + 1],
                                                                     in1=kv[:, 0:128], op0=ALU.mult, op1=ALU.add),
                             r=[Sc, EL, kv], w=[Sn])
                        cur = 1 - cur
                        k.op("act", lambda e: e.activation(out=OT[:, c0:c0 + 64], in_=op[:, 0:64], func=AF.Copy),
                             r=[op], w=[OT])
                for c5 in range(TB // 512):
                    sl = slice(c5 * 512, (c5 + 1) * 512)
                    k.op("act", lambda e: e.activation(out=sq[:, :], in_=OT[:, sl], func=AF.Square), r=[OT], w=[sq])
                    k.op("pe", lambda e: e.matmul(scp[:, :], lhsT=ones[:, :], rhs=sq[:, :], start=True, stop=True),
                         r=[ones, sq], w=[scp])
                    k.op("dve", lambda e: e.tensor_scalar(out=rs[:, :], in0=scp[:, :], scalar1=1.0 / 128, scalar2=EPS,
                                                          op0=ALU.mult, op1=ALU.add), r=[scp], w=[rs])
                    k.op("act", lambda e: e.activation(out=rs[:, :], in_=rs[:, :], func=AF.Sqrt), r=[rs], w=[rs])
                    k.op("dve", lambda e: e.reciprocal(out=rs[:, :], in_=rs[:, :]), r=[rs], w=[rs])
                    k.op("act", lambda e: e.activation(out=sil[:, :], in_=g[:, sl], func=AF.Silu), r=[g], w=[sil])
                    k.op("dve", lambda e: e.tensor_tensor(out=rs[:, :], in0=rs[:, :], in1=OT[:, sl], op=ALU.mult),
                         r=[rs, OT], w=[rs])
                    o = ob[c5 % 2]
                    k.op("dve", lambda e: e.scalar_tensor_tensor(out=o[:, :], in0=rs[:, :], scalar=gn[:, 0:1],
                                                                 in1=sil[:, :], op0=ALU.mult, op1=ALU.mult),
                         r=[rs, gn, sil], w=[o])
                    k.dma("sp", mixT.t[row0 + h * 128:row0 + (h + 1) * 128, t0 + c5 * 512:t0 + (c5 + 1) * 512], o[:, :],
                          r=[o], w=[mixT])
        k.barrier()


def extra_inputs(inp, l, hh):
    d = dict(hg_lb_param=np.ascontiguousarray(inp["hg_lb_param"][:, hh * 512:(hh + 1) * 512]),
             hg_norm_g=inp["hg_norm_g"][l])
    cs = slice(hh * 1024, (hh + 1) * 1024)
    mu = inp["rw_mu"][l]
    d["rw_mu"] = np.concatenate([mu[0:2048][cs], mu[2048:4096][cs], mu[4096:6144][cs], mu[6144:6400]])
    for n in ("rw_w0", "rw_a0", "rw_k_k", "rw_k_a", "rw_lnx_w", "rw_lnx_b"):
        d[n] = np.ascontiguousarray(inp[n][l][cs])
    d["rw_r_k"] = np.ascontiguousarray(inp["rw_r_k"][l].reshape(-1)[cs])
    for n in ("rw_w_w2", "rw_w_a2", "rw_w_g2"):
        d[n] = np.ascontiguousarray(inp[n][l][:, cs])
    return d


C0 = 0.6065306597126334
GN_EPS = 64e-5
RW_PARAMS = ["rw_mu", "rw_w0", "rw_w_w2", "rw_a0", "rw_w_a2", "rw_w_g2", "rw_k_k", "rw_k_a", "rw_r_k", "rw_lnx_w", "rw_lnx_b"]


class _Stop(Exception):
    pass


def _stop(n):
    import os as _os
    if int(_os.environ.get("RWSTOP", "99")) == n:
        _MUTE[0].mute = True


_MUTE = [None]


def phase_rw(k, cm, projT, T, P, mixT, row0, nlev=6):
    _MUTE[0] = k
    _phase_rw(k, cm, projT, T, P, mixT, row0, nlev)
    k.mute = False
    k.barrier()


def _phase_rw(k, cm, projT, T, P, mixT, row0, nlev=6):
    TB = 512
    NB = T // TB
    NCK = TB // 128
    with ExitStack() as es:
        idf, ones = cm["idf"], cm["ones"]
        hm = k.sb([128, 2], F32, "hm", es)
        k.op("pool", lambda e: e.memset(hm[:, :], 1.0), w=[hm])
        k.op("pool", lambda e: e.memset(hm[64:128, 0:1], 0.0), w=[hm])
        k.op("pool", lambda e: e.memset(hm[0:64, 1:2], 0.0), w=[hm])
        bones = k.sb([128, 128], F32, "bones", es)
        k.op("pool", lambda e: e.memset(bones[:, :], 1.0), w=[bones])
        k.op("pool", lambda e: e.memset(bones[0:64, 64:128], 0.0), w=[bones])
        k.op("pool", lambda e: e.memset(bones[64:128, 0:64], 0.0), w=[bones])
        mask2 = k.sb([128, 256], F32, "mask2", es)
        k.op("pool", lambda e: e.affine_select(out=mask2[:, 0:128], in_=ones[:, :], pattern=[[1, 128]], compare_op=ALU.is_gt,
                                               fill=0.0, base=0, channel_multiplier=-1), r=[ones], w=[mask2])
        k.op("pool", lambda e: e.affine_select(out=mask2[:, 128:256], in_=ones[:, :], pattern=[[1, 128]], compare_op=ALU.is_ge,
                                               fill=0.0, base=0, channel_multiplier=-1), r=[ones], w=[mask2])
        mls = mk_mask(k, es, [128, 128], [[-1, 128]], 0, 1, ALU.is_gt, "mls")
        rmask = k.sb([128, TB], F32, "rmask", es)
        k.op("pool", lambda e: e.memset(rmask[:, :], 1.0), w=[rmask])
        k.op("pool", lambda e: e.memset(rmask[:, :].rearrange("p (c j) -> p c j", j=128)[:, :, 0:1], 0.0), w=[rmask])
        def colparam(ap, name):
            t = k.sb([128, 8], F32, name, es)
            k.dma("sp", t[:, :], ap.rearrange("(h p) -> p h", p=128), w=[t], allow_slow_non_contiguous=True)
            return t
        mu = P["rw_mu"]
        mu_r, mu_k, mu_v = colparam(mu[0:1024], "mu_r"), colparam(mu[1024:2048], "mu_k"), colparam(mu[2048:3072], "mu_v")
        mu_l = k.sb([96, 3], F32, "mu_l", es)
        k.dma("sp", mu_l[0:96, 0:1], mu[3072:3168].rearrange("(p o) -> p o", o=1), w=[mu_l])
        k.dma("sp", mu_l[0:96, 1:2], mu[3168:3264].rearrange("(p o) -> p o", o=1), w=[mu_l])
        k.dma("sp", mu_l[0:64, 2:3], mu[3264:3328].rearrange("(p o) -> p o", o=1), w=[mu_l])
        w0c, a0c = colparam(P["rw_w0"], "w0c"), colparam(P["rw_a0"], "a0c")
        kkc, kac, rkc = colparam(P["rw_k_k"], "kkc"), colparam(P["rw_k_a"], "kac"), colparam(P["rw_r_k"], "rkc")
        lwc, lbc = colparam(P["rw_lnx_w"], "lwc"), colparam(P["rw_lnx_b"], "lbc")
        omk = k.sb([128, 8], F32, "omk", es)
        k.op("dve", lambda e: e.tensor_scalar(out=omk[:, :], in0=kac[:, :], scalar1=-1.0, scalar2=1.0, op0=ALU.mult,
                                              op1=ALU.add), r=[kac], w=[omk])
        ww2 = k.sb([96, 1024], F32, "ww2", es)
        wa2 = k.sb([96, 1024], F32, "wa2", es)
        wg2 = k.sb([64, 1024], F32, "wg2", es)
        k.dma("sp", ww2[:, :], P["rw_w_w2"], w=[ww2])
        k.dma("sp", wa2[:, :], P["rw_w_a2"], w=[wa2])
        k.dma("sp", wg2[:, :], P["rw_w_g2"], w=[wg2])
        def T2(name, shape=None):
            return k.sb(shape or [128, TB], F32, name, es)
        lraw = [T2("lraw%d" % i, [96, TB + 1]) for i in range(3)]
        ltmp = T2("ltmp", [96, TB])
        tanhT, asT, sigg = T2("tanhT", [96, TB]), T2("asT", [96, TB]), T2("sigg", [64, TB])
        raw = [T2("raw%d" % i, [128, TB + 1]) for i in range(3)]
        names = ["rs_", "ks_", "vs_", "sgw", "asig", "G", "cs", "gi", "gm", "gp", "kk", "tmp", "kmod", "AT", "ka", "BT",
                 "KT", "RT", "BH", "KHT", "BON", "YT", "cen", "tmp2"]
        t_ = {n: T2(n) for n in names}
        AR = [T2("AR%d" % h, [128, NCK, 256]) for h in range(2)]
        Vt, BHt, KHt = T2("Vt", [128, NCK, 128]), T2("BHt", [128, NCK, 128]), T2("KHt", [128, NCK, 128])
        Vpad = T2("Vpad", [128, NCK, 2, 128])
        k.op("pool", lambda e: e.memset(Vpad[:, :, :, :], 0.0), w=[Vpad])
        NSET = 2
        AB = [[T2("AB", [128, 256]) for _ in range(2)] for _ in range(NSET)]
        AK = [[T2("AK", [128, 256]) for _ in range(2)] for _ in range(NSET)]
        TTf = [[T2("TTf", [128, 128]) for _ in range(2)] for _ in range(NSET)]
        Mb = [T2("Mb", [128, 128]) for _ in range(2)]
        Nb = [T2("Nb", [128, 128]) for _ in range(2)]
        TTb = [T2("TTb", [128, 128]) for _ in range(2)]
        BD = [[T2("BD", [128, 128]) for _ in range(2)] for _ in range(8)]
        bdcur = [0] * 8
        for hp in range(8):
            k.op("pool", lambda e: e.memset(BD[hp][0][:, :], 0.0), w=[BD[hp][0]])
        X = T2("X", [128, 128])
        Pm = T2("Pm", [128, 128])
        Pp = T2("Pp", [128, 2, 128])
        k.op("pool", lambda e: e.memset(Pp[:, :, :], 0.0), w=[Pp])
        ob = [T2("ob") for _ in range(2)]
        pp = [k.ps([128, 512], F32, "pp", es) for _ in range(3)]
        ip = [k.ps([128, 512], F32, "ip", es) for _ in range(3)]
        cpx, cpy = k.ps([128, 512], F32, "cpx", es), k.ps([128, 512], F32, "cpy", es)
        cnt = {"pp": 0, "ev": 0, "unit": 0}

        def nextpp():
            cnt["pp"] += 1
            return pp[cnt["pp"] % 3]

        def ev(out_ap, in_ap, r, w, engines=("act", "dve")):
            cnt["ev"] += 1
            evac(k, cnt["ev"], out_ap, in_ap, r, w, engines)

        def tshift(rawt, rows_ap_fn, blk, mu_ap, out, np_):
            t0 = blk * TB
            if blk == 0:
                k.op("pool", lambda e: e.memset(rawt[0:np_, 0:1], 0.0), w=[rawt])
                k.dma("sp", rawt[0:np_, 1:TB + 1], rows_ap_fn(0, TB), r=[projT], w=[rawt])
            else:
                k.dma("sp", rawt[0:np_, 0:TB + 1], rows_ap_fn(t0 - 1, t0 + TB), r=[projT], w=[rawt])
            tm = t_["tmp2"] if np_ == 128 else ltmp
            k.op("pool", lambda e: e.tensor_tensor(out=tm[0:np_, :], in0=rawt[0:np_, 0:TB], in1=rawt[0:np_, 1:TB + 1],
                                                   op=ALU.subtract), r=[rawt], w=[tm])
            k.op("dve", lambda e: e.scalar_tensor_tensor(out=out[0:np_, :], in0=tm[0:np_, :], scalar=mu_ap,
                                                         in1=rawt[0:np_, 1:TB + 1], op0=ALU.mult, op1=ALU.add),
                 r=[tm, rawt], w=[out])

        import os as _os
        _b0, _b1 = [int(v) for v in _os.environ.get('RWBLK', '0,%d' % NB).split(',')]
        for blk in range(_b0, _b1):
            t0 = blk * TB
            tshift(lraw[0], lambda a, b: projT.t[O_RWW:O_RWW + 96, a:b], blk, mu_l[0:96, 0:1], tanhT, 96)
            k.op("act", lambda e: e.activation(out=tanhT[:, :], in_=tanhT[:, :], func=AF.Tanh), r=[tanhT], w=[tanhT])
            tshift(lraw[1], lambda a, b: projT.t[O_RWA:O_RWA + 96, a:b], blk, mu_l[0:96, 1:2], asT, 96)
            tshift(lraw[2], lambda a, b: projT.t[O_RWG:O_RWG + 64, a:b], blk, mu_l[0:64, 2:3], sigg, 64)
            k.op("act", lambda e: e.activation(out=sigg[:, :], in_=sigg[:, :], func=AF.Sigmoid), r=[sigg], w=[sigg])
            _stop(1)
            for hp in range(8):
                cs_ = slice(hp * 128, (hp + 1) * 128)
                hc = slice(hp, hp + 1)
                rs_, ks_, vs_ = t_["rs_"], t_["ks_"], t_["vs_"]
                tshift(raw[0], lambda a, b: projT.t[O_RWR + hp * 128:O_RWR + (hp + 1) * 128, a:b], blk, mu_r[:, hc], rs_, 128)
                tshift(raw[1], lambda a, b: projT.t[O_RWK + hp * 128:O_RWK + (hp + 1) * 128, a:b], blk, mu_k[:, hc], ks_, 128)
                tshift(raw[2], lambda a, b: projT.t[O_RWV + hp * 128:O_RWV + (hp + 1) * 128, a:b], blk, mu_v[:, hc], vs_, 128)
                _stop(2)
                sgw, asig, G, cs = t_["sgw"], t_["asig"], t_["G"], t_["cs"]
                gi, gm, gp, kk, tmp, kmod = t_["gi"], t_["gm"], t_["gp"], t_["kk"], t_["tmp"], t_["kmod"]
                AT, ka, BT, KT, RT, BH, KHT, BON = (t_[n] for n in ["AT", "ka", "BT", "KT", "RT", "BH", "KHT", "BON"])
                YT, cen = t_["YT"], t_["cen"]
                p = nextpp()
                k.op("pe", lambda e: e.matmul(p[:, :], lhsT=ww2[:, cs_], rhs=tanhT[:, :], start=True, stop=True),
                     r=[ww2, tanhT], w=[p])
                k.op("act", lambda e: e.activation(out=sgw[:, :], in_=p[:, :], func=AF.Sigmoid, bias=w0c[:, hc]),
                     r=[p, w0c], w=[sgw])
                p = nextpp()
                k.op("pe", lambda e: e.matmul(p[:, :], lhsT=wa2[:, cs_], rhs=asT[:, :], start=True, stop=True),
                     r=[wa2, asT], w=[p])
                k.op("act", lambda e: e.activation(out=asig[:, :], in_=p[:, :], func=AF.Sigmoid, bias=a0c[:, hc]),
                     r=[p, a0c], w=[asig])
                p = nextpp()
                k.op("pe", lambda e: e.matmul(p[:, :], lhsT=wg2[:, cs_], rhs=sigg[:, :], start=True, stop=True),
                     r=[wg2, sigg], w=[p])
                k.op("act", lambda e: e.activation(out=G[:, :], in_=p[:, :], func=AF.Copy), r=[p], w=[G])
                k.op("dve", lambda e: e.tensor_tensor_scan(out=cs[:, :], data0=rmask[:, :], data1=sgw[:, :], initial=0.0,
                                                           op0=ALU.mult, op1=ALU.add), r=[rmask, sgw], w=[cs])
                k.op("act", lambda e: e.activation(out=gi[:, :], in_=cs[:, :], func=AF.Exp, scale=C0), r=[cs], w=[gi])
                k.op("act", lambda e: e.activation(out=gm[:, :], in_=cs[:, :], func=AF.Exp, scale=-C0), r=[cs], w=[gm])
                k.op("pool", lambda e: e.tensor_tensor(out=tmp[:, :], in0=cs[:, :], in1=sgw[:, :], op=ALU.subtract),
                     r=[cs, sgw], w=[tmp])
                k.op("act", lambda e: e.activation(out=gp[:, :], in_=tmp[:, :], func=AF.Exp, scale=-C0), r=[tmp], w=[gp])
                k.op("dve", lambda e: e.tensor_scalar(out=kk[:, :], in0=ks_[:, :], scalar1=kkc[:, hc], scalar2=None,
                                                      op0=ALU.mult), r=[ks_, kkc], w=[kk])
                k.op("pool", lambda e: e.tensor_tensor(out=tmp[:, :], in0=kk[:, :], in1=kk[:, :], op=ALU.mult), r=[kk], w=[tmp])
                p = nextpp()
                k.op("pe", lambda e: e.matmul(p[:, :], lhsT=bones[:, :], rhs=tmp[:, :], start=True, stop=True),
                     r=[bones, tmp], w=[p])
                k.op("dve", lambda e: e.tensor_scalar_max(out=tmp[:, :], in0=p[:, :], scalar1=1e-24), r=[p], w=[tmp])
                k.op("act", lambda e: e.activation(out=tmp[:, :], in_=tmp[:, :], func=AF.Sqrt), r=[tmp], w=[tmp])
                k.op("dve", lambda e: e.reciprocal(out=tmp[:, :], in_=tmp[:, :]), r=[tmp], w=[tmp])
                k.op("dve", lambda e: e.tensor_tensor(out=kk[:, :], in0=kk[:, :], in1=tmp[:, :], op=ALU.mult), r=[kk, tmp], w=[kk])
                k.op("pool", lambda e: e.tensor_scalar(out=kmod[:, :], in0=asig[:, :], scalar1=kac[:, hc], scalar2=omk[:, hc],
                                                       op0=ALU.mult, op1=ALU.add), r=[asig, kac, omk], w=[kmod])
                k.op("pool", lambda e: e.tensor_tensor(out=kmod[:, :], in0=kmod[:, :], in1=ks_[:, :], op=ALU.mult),
                     r=[kmod, ks_], w=[kmod])
                k.op("dve", lambda e: e.scalar_tensor_tensor(out=AT[:, :], in0=kk[:, :], scalar=-1.0, in1=gp[:, :],
                                                             op0=ALU.mult, op1=ALU.mult), r=[kk, gp], w=[AT])
                k.op("pool", lambda e: e.tensor_tensor(out=ka[:, :], in0=kk[:, :], in1=asig[:, :], op=ALU.mult),
                     r=[kk, asig], w=[ka])
                k.op("dve", lambda e: e.tensor_tensor(out=BT[:, :], in0=ka[:, :], in1=gi[:, :], op=ALU.mult), r=[ka, gi], w=[BT])
                k.op("pool", lambda e: e.tensor_tensor(out=KT[:, :], in0=kmod[:, :], in1=gi[:, :], op=ALU.mult),
                     r=[kmod, gi], w=[KT])
                k.op("dve", lambda e: e.tensor_tensor(out=RT[:, :], in0=rs_[:, :], in1=gm[:, :], op=ALU.mult), r=[rs_, gm], w=[RT])
                gm3 = gm[:, :].rearrange("p (c j) -> p c j", j=128)
                glb = gm3[:, :, 127:128].to_broadcast([128, NCK, 128])
                k.op("dve", lambda e: e.tensor_tensor(out=BH[:, :].rearrange("p (c j) -> p c j", j=128),
                                                      in0=BT[:, :].rearrange("p (c j) -> p c j", j=128), in1=glb,
                                                      op=ALU.mult), r=[BT, gm], w=[BH])
                k.op("pool", lambda e: e.tensor_tensor(out=KHT[:, :].rearrange("p (c j) -> p c j", j=128),
                                                       in0=KT[:, :].rearrange("p (c j) -> p c j", j=128), in1=glb,
                                                       op=ALU.mult), r=[KT, gm], w=[KHT])
                k.op("dve", lambda e: e.scalar_tensor_tensor(out=tmp[:, :], in0=rs_[:, :], scalar=rkc[:, hc], in1=kmod[:, :],
                                                             op0=ALU.mult, op1=ALU.mult), r=[rs_, rkc, kmod], w=[tmp])
                p = nextpp()
                k.op("pe", lambda e: e.matmul(p[:, :], lhsT=bones[:, :], rhs=tmp[:, :], start=True, stop=True),
                     r=[bones, tmp], w=[p])
                k.op("dve", lambda e: e.tensor_tensor(out=BON[:, :], in0=p[:, :], in1=vs_[:, :], op=ALU.mult), r=[p, vs_], w=[BON])
                _stop(3)
                for h in range(2):
                    eng = "dve" if h == 0 else "pool"
                    k.op(eng, lambda e: e.tensor_scalar(out=AR[h][:, :, 0:128], in0=AT[:, :].rearrange("p (c j) -> p c j", j=128),
                                                        scalar1=hm[:, h:h + 1], scalar2=None, op0=ALU.mult),
                         r=[AT, hm], w=[AR[h]])
                    k.op(eng, lambda e: e.tensor_scalar(out=AR[h][:, :, 128:256], in0=RT[:, :].rearrange("p (c j) -> p c j", j=128),
                                                        scalar1=hm[:, h:h + 1], scalar2=None, op0=ALU.mult),
                         r=[RT, hm], w=[AR[h]])
                for (src, dst) in ((vs_, Vt), (BH, BHt), (KHT, KHt)):
                    p = nextpp()
                    for c in range(NCK):
                        k.op("pe", lambda e: e.transpose(out=p[:, c * 128:(c + 1) * 128], in_=src[:, c * 128:(c + 1) * 128],
                                                         identity=idf[:]), r=[src, idf], w=[p])
                    ev(dst[:, :, :], p[:, :].rearrange("p (c j) -> p c j", j=128), [p], [dst])
                    if dst is Vt:
                        for h in range(2):
                            ev(Vpad[:, :, h, h * 64:(h + 1) * 64],
                               p[:, :].rearrange("p (c j) -> p c j", j=128)[:, :, h * 64:(h + 1) * 64], [p], [Vpad])
                _stop(4)
                for c in range(NCK):
                    sl = slice(c * 128, (c + 1) * 128)
                    st_ = cnt["unit"] % NSET
                    cnt["unit"] += 1
                    for h in range(2):
                        p1 = nextpp()
                        k.op("pe", lambda e: e.matmul(p1[:, 0:256], lhsT=BT[:, sl], rhs=AR[h][:, c, :], start=True, stop=True),
                             r=[BT, AR[h]], w=[p1])
                        k.op("dve", lambda e: e.tensor_tensor(out=AB[st_][h][:, :], in0=p1[:, 0:256], in1=mask2[:, :],
                                                              op=ALU.mult), r=[p1, mask2], w=[AB[st_][h]])
                        p2 = nextpp()
                        k.op("pe", lambda e: e.matmul(p2[:, 0:256], lhsT=KT[:, sl], rhs=AR[h][:, c, :], start=True, stop=True),
                             r=[KT, AR[h]], w=[p2])
                        k.op("dve", lambda e: e.tensor_tensor(out=AK[st_][h][:, :], in0=p2[:, 0:256], in1=mask2[:, :],
                                                              op=ALU.mult), r=[p2, mask2], w=[AK[st_][h]])
                        p3 = nextpp()
                        k.op("pe", lambda e: e.matmul(p3[:, 0:128], lhsT=AR[h][:, c, 0:128], rhs=BT[:, sl], start=True,
                                                      stop=True), r=[BT, AR[h]], w=[p3])
                        M, N_ = Mb[0], Nb[0]
                        k.op("dve", lambda e: e.tensor_tensor(out=N_[:, :], in0=p3[:, 0:128], in1=mls[:, :], op=ALU.mult),
                             r=[p3, mls], w=[N_])
                        k.op("pool", lambda e: e.tensor_copy(out=M[:, :], in_=AB[st_][h][:, 0:128]), r=[AB[st_][h]], w=[M])
                        TT = TTb[0]
                        k.op("pool", lambda e: e.tensor_tensor(out=TT[:, :], in0=AB[st_][h][:, 0:128], in1=idf[:, :],
                                                               op=ALU.add), r=[AB[st_][h], idf], w=[TT])
                        cur = 0
                        for lev in range(nlev):
                            lastl = lev == nlev - 1
                            M, N_, TT = Mb[cur], Nb[cur], TTb[cur]
                            M2, N2 = Mb[1 - cur], Nb[1 - cur]
                            TT2 = TTf[st_][h] if lastl else TTb[1 - cur]
                            k.op("pe", lambda e: e.matmul(ip[0][:, 0:128], lhsT=M[:, :], rhs=N_[:, :], start=True, stop=True),
                                 r=[M, N_], w=[ip[0]])
                            if not lastl:
                                k.op("pe", lambda e: e.matmul(ip[1][:, 0:128], lhsT=N_[:, :], rhs=M[:, :], start=True,
                                                              stop=True), r=[M, N_], w=[ip[1]])
                            k.op("act", lambda e: e.activation(out=N2[:, :], in_=ip[0][:, 0:128], func=AF.Copy),
                                 r=[ip[0]], w=[N2])
                            if not lastl:
                                k.op("dve", lambda e: e.tensor_copy(out=M2[:, :], in_=ip[1][:, 0:128]), r=[ip[1]], w=[M2])
                            k.op("pe", lambda e: e.matmul(ip[2][:, 0:128], lhsT=N2[:, :], rhs=TT[:, :], start=True, stop=True),
                                 r=[N2, TT], w=[ip[2]])
                            k.op("dve", lambda e: e.tensor_tensor(out=TT2[:, :], in0=ip[2][:, 0:128], in1=TT[:, :],
                                                                  op=ALU.add), r=[ip[2], TT], w=[TT2])
                            cur = 1 - cur
                    _stop(5)
                    bd = BD[hp][bdcur[hp]]
                    bdn = BD[hp][1 - bdcur[hp]]
                    bdcur[hp] = 1 - bdcur[hp]
                    k.op("pe", lambda e: e.matmul(cpx[:, 0:128], lhsT=AT[:, sl], rhs=bd[:, :], start=True, stop=False),
                         r=[AT, bd], w=[cpx])
                    for h in range(2):
                        hs = slice(h * 64, (h + 1) * 64)
                        k.op("pe", lambda e: e.matmul(cpx[:, hs], lhsT=AK[st_][h][:, 0:128], rhs=Vt[:, c, hs], start=False,
                                                      stop=(h == 1)), r=[AK[st_][h], Vt], w=[cpx])
                    k.op("act", lambda e: e.activation(out=X[:, :], in_=cpx[:, 0:128], func=AF.Copy), r=[cpx], w=[X])
                    for h in range(2):
                        hs = slice(h * 64, (h + 1) * 64)
                        k.op("pe", lambda e: e.matmul(cpx[:, hs], lhsT=TTf[st_][h][:, :], rhs=X[:, hs], start=True, stop=True),
                             r=[TTf[st_][h], X], w=[cpx])
                    k.op("act", lambda e: e.activation(out=Pm[:, :], in_=cpx[:, 0:128], func=AF.Copy), r=[cpx], w=[Pm])
                    for h in range(2):
                        hs = slice(h * 64, (h + 1) * 64)
                        k.op("dve", lambda e: e.tensor_copy(out=Pp[:, h, hs], in_=cpx[:, hs]), r=[cpx], w=[Pp])
                    k.op("pe", lambda e: e.matmul(cpy[:, 0:128], lhsT=bd[:, :], rhs=RT[:, sl], start=True, stop=False),
                         r=[bd, RT], w=[cpy])
                    for h in range(2):
                        k.op("pe", lambda e: e.matmul(cpy[:, 0:128], lhsT=Pp[:, h, :], rhs=AB[st_][h][:, 128:256], start=False,
                                                      stop=False), r=[Pp, AB[st_][h]], w=[cpy])
                        k.op("pe", lambda e: e.matmul(cpy[:, 0:128], lhsT=Vpad[:, c, h, :], rhs=AK[st_][h][:, 128:256],
                                                      start=False, stop=(h == 1)), r=[Vpad, AK[st_][h]], w=[cpy])
                    k.op("act", lambda e: e.activation(out=YT[:, sl], in_=cpy[:, 0:128], func=AF.Copy), r=[cpy], w=[YT])
                    k.op("pe", lambda e: e.matmul(cpx[:, 0:128], lhsT=BHt[:, c, :], rhs=Pm[:, :], start=True, stop=False),
                         r=[BHt, Pm], w=[cpx])
                    k.op("pe", lambda e: e.matmul(cpx[:, 0:128], lhsT=KHt[:, c, :], rhs=Vt[:, c, :], start=False, stop=True),
                         r=[KHt, Vt], w=[cpx])
                    k.op("dve", lambda e: e.scalar_tensor_tensor(out=bdn[:, :], in0=bd[:, :], scalar=gm[:, c * 128 + 127:c * 128 + 128],
                                                                 in1=cpx[:, 0:128], op0=ALU.mult, op1=ALU.add),
                         r=[bd, gm, cpx], w=[bdn])
                    k.op("dve", lambda e: e.tensor_tensor(out=bdn[:, :], in0=bdn[:, :], in1=bones[:, :], op=ALU.mult),
                         r=[bdn, bones], w=[bdn])
                _stop(6)
                p = nextpp()
                k.op("pe", lambda e: e.matmul(p[:, :], lhsT=bones[:, :], rhs=YT[:, :], start=True, stop=True), r=[bones, YT], w=[p])
                k.op("dve", lambda e: e.scalar_tensor_tensor(out=cen[:, :], in0=p[:, :], scalar=-1.0 / 64, in1=YT[:, :],
                                                             op0=ALU.mult, op1=ALU.add), r=[p, YT], w=[cen])
                k.op("act", lambda e: e.activation(out=tmp[:, :], in_=cen[:, :], func=AF.Square), r=[cen], w=[tmp])
                p = nextpp()
                k.op("pe", lambda e: e.matmul(p[:, :], lhsT=bones[:, :], rhs=tmp[:, :], start=True, stop=True), r=[bones, tmp], w=[p])
                k.op("dve", lambda e: e.tensor_scalar(out=tmp[:, :], in0=p[:, :], scalar1=1.0 / 64, scalar2=GN_EPS, op0=ALU.mult,
                                                      op1=ALU.add), r=[p], w=[tmp])
                k.op("act", lambda e: e.activation(out=tmp[:, :], in_=tmp[:, :], func=AF.Sqrt), r=[tmp], w=[tmp])
                k.op("dve", lambda e: e.reciprocal(out=tmp[:, :], in_=tmp[:, :]), r=[tmp], w=[tmp])
                k.op("dve", lambda e: e.tensor_tensor(out=cen[:, :], in0=cen[:, :], in1=tmp[:, :], op=ALU.mult), r=[cen, tmp], w=[cen])
                k.op("pool", lambda e: e.tensor_scalar(out=cen[:, :], in0=cen[:, :], scalar1=lwc[:, hc], scalar2=lbc[:, hc],
                                                       op0=ALU.mult, op1=ALU.add), r=[cen, lwc, lbc], w=[cen])
                k.op("pool", lambda e: e.tensor_tensor(out=cen[:, :], in0=cen[:, :], in1=BON[:, :], op=ALU.add), r=[cen, BON], w=[cen])
                o = ob[hp % 2]
                k.op("dve", lambda e: e.tensor_tensor(out=o[:, :], in0=cen[:, :], in1=G[:, :], op=ALU.mult), r=[cen, G], w=[o])
                k.dma("sp", mixT.t[row0 + hp * 128:row0 + (hp + 1) * 128, t0:t0 + TB], o[:, :], r=[o], w=[mixT])
        k.barrier()


def _core_cols(hh):
    c = []
    for base in (0, 1024, 2048, 3072, 4096, 5120, 6144):
        c += list(range(base + hh * 512, base + (hh + 1) * 512))
    rb = 7168
    for base in (0, 2048, 4096):
        c += list(range(rb + base + hh * 1024, rb + base + (hh + 1) * 1024))
    c += list(range(rb + 6144, rb + 6400))
    return np.array(c)


def _a_inputs(inp, l, hh, xb):
    cols = _core_cols(hh)
    d = dict(x=xb, norm1_g=inp["norm1_g"][l], w_in=np.ascontiguousarray(inp["w_in"][l][:, cols]),
             sb_norm_g=np.ascontiguousarray(inp["sb_norm_g"][l][hh * 512:(hh + 1) * 512]))
    d.update(extra_inputs(inp, l, hh))
    return {k_: np.ascontiguousarray(v, dtype=np.float32) for k_, v in d.items()}


def kernel(**inputs):
    inp = {k_: np.asarray(v, dtype=np.float32) for k_, v in inputs.items()}
    x = np.ascontiguousarray(inp["x"])
    NB_, T, _ = x.shape
    NTH = T // 2
    for l in range(2):
        nca = bass.Bass("TRN2", target_bir_lowering=False)
        build_A(nca, T, l)
        maps = [_a_inputs(inp, l, c % 2, x[c // 2]) for c in range(8)]
        res = run_bass_kernel_spmd(nca, maps, core_ids=list(range(8)))
        mixT = np.empty((NB_, D, T), np.float32)
        for c in range(8):
            b, hh = c // 2, c % 2
            r = res.results[c]["mixT"]
            mixT[b, hh * 512:(hh + 1) * 512] = r[0:512]
            mixT[b, 1024 + hh * 512:1024 + (hh + 1) * 512] = r[512:1024]
            mixT[b, 2048 + hh * 1024:2048 + (hh + 1) * 1024] = r[1024:2048]
        del res
        ncb = bass.Bass("TRN2", target_bir_lowering=False)
        build_B(ncb, NTH, l == 1)
        maps = []
        for c in range(8):
            b, th = c // 2, c % 2
            ts = slice(th * NTH, (th + 1) * NTH)
            maps.append(dict(x=np.ascontiguousarray(x[b, ts]), mixT=np.ascontiguousarray(mixT[b][:, ts]),
                             w_out=inp["w_out"][l], norm2_g=inp["norm2_g"][l], w_ff_in=inp["w_ff_in"][l],
                             w_ff_out=inp["w_ff_out"][l], final_g=inp["final_g"]))
        res = run_bass_kernel_spmd(ncb, maps, core_ids=list(range(8)))
        xn = np.empty_like(x)
        for c in range(8):
            b, th = c // 2, c % 2
            xn[b, th * NTH:(th + 1) * NTH] = res.results[c]["y"]
        x = xn
        del res
    return x
```
